# Optimizing a Trainium2 kernel written in Bass

```python
import jax, jax.numpy as jnp
from jax import lax
import numpy as np

D_MODEL = 2048
BATCH = 8
SEQ = 2048
DEPTH = 2

A_HEADS = 16
A_KV_HEADS = 4
A_HEAD_DIM = 64
A_WIDTH = A_HEADS * A_HEAD_DIM
A_KV_WIDTH = A_KV_HEADS * A_HEAD_DIM
WINDOW = 128
BLOCK = 128
ROPE_DIM = A_HEAD_DIM // 4
ROPE_THETA = 500000.0
B_HEADS = 4
B_KEY_DIM = 128
B_VAL_DIM = 256
B_K_WIDTH = B_HEADS * B_KEY_DIM
B_V_WIDTH = B_HEADS * B_VAL_DIM
GATE_RANK = 16
GATE_NORMALIZER = 16.0
CHUNK = 64
D_FF = 5632
CONV_WIDTH = 3
EPS = 1e-6

IN_SIZES = (A_WIDTH, A_KV_WIDTH, A_KV_WIDTH, B_K_WIDTH, B_K_WIDTH, B_V_WIDTH,
            B_V_WIDTH, GATE_RANK, D_MODEL, D_MODEL)
N_IN = sum(IN_SIZES)

kernel_name = "hybrid_swa_sink_gla_convffn"


def rms_norm(x, g):
    xf = x.astype(jnp.float32)
    y = xf * lax.rsqrt(jnp.mean(xf * xf, axis=-1, keepdims=True) + EPS)
    return (y * g.astype(jnp.float32)).astype(x.dtype)


def partial_rope(x, positions):
    half = ROPE_DIM // 2
    inv_freq = ROPE_THETA ** (-jnp.arange(half, dtype=jnp.float32) * 2.0 / ROPE_DIM)
    ang = positions.astype(jnp.float32)[..., None] * inv_freq
    cos = jnp.cos(ang)[:, :, None, :]
    sin = jnp.sin(ang)[:, :, None, :]
    x1 = x[..., :half].astype(jnp.float32)
    x2 = x[..., half:ROPE_DIM].astype(jnp.float32)
    r1 = (x1 * cos - x2 * sin).astype(x.dtype)
    r2 = (x2 * cos + x1 * sin).astype(x.dtype)
    return jnp.concatenate([r1, r2, x[..., ROPE_DIM:]], axis=-1)


def swa_with_sinks(q, k, v, sink):
    Bn, S = q.shape[0], q.shape[1]
    nb = S // BLOCK
    G = A_HEADS // A_KV_HEADS
    qb = q.reshape(Bn, nb, BLOCK, A_KV_HEADS, G, A_HEAD_DIM)

    def band(t):
        tb = t.reshape(Bn, nb, BLOCK, A_KV_HEADS, A_HEAD_DIM)
        prev = jnp.pad(tb, ((0, 0), (1, 0), (0, 0), (0, 0), (0, 0)))[:, :-1]
        return jnp.concatenate([prev, tb], axis=2)

    kb, vb = band(k), band(v)
    s = jnp.einsum('bnqhgd,bnkhd->bnhgqk', qb, kb,
                   preferred_element_type=jnp.float32) * (A_HEAD_DIM ** -0.5)
    qi = jnp.arange(BLOCK)[:, None]
    ki = jnp.arange(2 * BLOCK)[None, :]
    rel = qi + BLOCK - ki
    in_win = (rel >= 0) & (rel < WINDOW)
    blk = jnp.arange(nb)[:, None, None]
    valid = in_win[None] & ((blk > 0) | (ki[None] >= BLOCK))
    s = jnp.where(valid[None, :, None, None], s, -jnp.inf)
    sink_l = sink.astype(jnp.float32).reshape(A_KV_HEADS, G)[None, None, :, :, None, None]
    m = jnp.maximum(jnp.max(s, axis=-1, keepdims=True), sink_l)
    p = jnp.exp(s - m)
    p = p / (jnp.sum(p, axis=-1, keepdims=True) + jnp.exp(sink_l - m))
    o = jnp.einsum('bnhgqk,bnkhd->bnqhgd', p.astype(v.dtype), vb)
    return o.reshape(Bn, S, A_WIDTH)


def gla_chunked(q, k, v, log_a):
    Bn, S, H, dk = q.shape
    dv = v.shape[-1]
    nc = S // CHUNK
    f32 = jnp.float32

    def chunk(t):
        return t.astype(f32).reshape(Bn, nc, CHUNK, H, t.shape[-1])

    qc = chunk(q) * (dk ** -0.5)
    kc = chunk(k)
    vc = chunk(v)
    b = jnp.cumsum(chunk(log_a), axis=2)
    b_last = b[:, :, -1:]
    q_dec = qc * jnp.exp(b)
    k_inv = kc * jnp.exp(-b)
    k_end = kc * jnp.exp(b_last - b)
    causal = jnp.tril(jnp.ones((CHUNK, CHUNK), dtype=bool))
    attn = jnp.einsum('bclhd,bcshd->bchls', q_dec, k_inv)
    attn = jnp.where(causal, attn, 0.0)
    o_intra = jnp.einsum('bchls,bcshe->bclhe', attn, vc)
    dstate = jnp.einsum('bcshd,bcshe->bchde', k_end, vc)
    decay = jnp.exp(b_last[:, :, 0])

    def step(state, inp):
        dec, ds = inp
        return dec[..., None] * state + ds, state

    s0 = jnp.zeros((Bn, H, dk, dv), f32)
    _, s_prev = lax.scan(step, s0, (jnp.moveaxis(decay, 1, 0), jnp.moveaxis(dstate, 1, 0)))
    o_inter = jnp.einsum('bclhd,cbhde->bclhe', q_dec, s_prev)
    return (o_intra + o_inter).reshape(Bn, S, H, dv).astype(v.dtype)


def causal_depthwise_conv(h, w, b):
    S = h.shape[1]
    hp = jnp.pad(h, ((0, 0), (CONV_WIDTH - 1, 0), (0, 0)))
    out = b
    for j in range(CONV_WIDTH):
        out = out + w[j] * hp[:, j:j + S]
    return out


def setup_inputs(seed: int = 0) -> dict:
    key = jax.random.key(seed)
    ks = jax.random.split(key, 20)
    f32 = jnp.float32
    nrm = lambda k, shape, scale: jax.random.normal(k, shape, f32) * scale
    x = jax.random.normal(ks[0], (BATCH, SEQ, D_MODEL), f32)
    start = jax.random.randint(ks[1], (BATCH, 1), 0, 4096, dtype=jnp.int32)
    positions = (start + jnp.arange(SEQ, dtype=jnp.int32)[None, :]).astype(jnp.int32)
    return {
        "x": x,
        "positions": positions,
        "norm_mix_g": 1.0 + nrm(ks[2], (DEPTH, D_MODEL), 0.02),
        "w_in": nrm(ks[3], (DEPTH, D_MODEL, N_IN), D_MODEL ** -0.5),
        "sink": nrm(ks[4], (DEPTH, A_HEADS), 0.5),
        "w_alpha_up": nrm(ks[5], (DEPTH, GATE_RANK, B_K_WIDTH), GATE_RANK ** -0.5),
        "b_alpha": nrm(ks[6], (DEPTH, B_K_WIDTH), 0.01),
        "gla_norm_g": 1.0 + nrm(ks[7], (DEPTH, B_VAL_DIM), 0.02),
        "w_branch_a": nrm(ks[8], (DEPTH, A_WIDTH, D_MODEL), A_WIDTH ** -0.5),
        "w_branch_b": nrm(ks[9], (DEPTH, B_V_WIDTH, D_MODEL), B_V_WIDTH ** -0.5),
        "w_out": nrm(ks[10], (DEPTH, D_MODEL, D_MODEL), D_MODEL ** -0.5),
        "norm_ffn_g": 1.0 + nrm(ks[11], (DEPTH, D_MODEL), 0.02),
        "w_up": nrm(ks[12], (DEPTH, D_MODEL, 2 * D_FF), D_MODEL ** -0.5),
        "conv_w": nrm(ks[13], (DEPTH, CONV_WIDTH, 2 * D_FF), CONV_WIDTH ** -0.5),
        "conv_b": nrm(ks[14], (DEPTH, 2 * D_FF), 0.01),
        "w_down": nrm(ks[15], (DEPTH, D_FF, D_MODEL), D_FF ** -0.5),
        "final_g": 1.0 + nrm(ks[16], (D_MODEL,), 0.02),
    }


def reference(x, positions, norm_mix_g, w_in, sink, w_alpha_up, b_alpha, gla_norm_g,
              w_branch_a, w_branch_b, w_out, norm_ffn_g, w_up, conv_w, conv_b, w_down,
              final_g):
    Bn, S = x.shape[0], x.shape[1]
    offsets = np.cumsum(IN_SIZES)[:-1].tolist()
    for l in range(DEPTH):
        xn = rms_norm(x, norm_mix_g[l])
        z = xn @ w_in[l]
        qa, ka, va, qb, kb, vb, rb, alpha_lr, gate_a, gate_b = jnp.split(z, offsets, axis=-1)
        qa = partial_rope(qa.reshape(Bn, S, A_HEADS, A_HEAD_DIM), positions)
        ka = partial_rope(ka.reshape(Bn, S, A_KV_HEADS, A_HEAD_DIM), positions)
        va = va.reshape(Bn, S, A_KV_HEADS, A_HEAD_DIM)
        ya = swa_with_sinks(qa, ka, va, sink[l])
        log_a = jax.nn.log_sigmoid((alpha_lr @ w_alpha_up[l] + b_alpha[l]).astype(jnp.float32)) / GATE_NORMALIZER
        ob = gla_chunked(qb.reshape(Bn, S, B_HEADS, B_KEY_DIM),
                         kb.reshape(Bn, S, B_HEADS, B_KEY_DIM),
                         vb.reshape(Bn, S, B_HEADS, B_VAL_DIM),
                         log_a.reshape(Bn, S, B_HEADS, B_KEY_DIM))
        ob = rms_norm(ob, gla_norm_g[l]) * jax.nn.silu(rb.reshape(Bn, S, B_HEADS, B_VAL_DIM))
        yb = ob.reshape(Bn, S, B_V_WIDTH)
        mixed = (jax.nn.sigmoid(gate_a) * (ya @ w_branch_a[l])
                 + jax.nn.sigmoid(gate_b) * (yb @ w_branch_b[l]))
        x = x + mixed @ w_out[l]
        h = rms_norm(x, norm_ffn_g[l]) @ w_up[l]
        h = causal_depthwise_conv(h, conv_w[l], conv_b[l])
        g, u = jnp.split(h, 2, axis=-1)
        x = x + (jax.nn.silu(g) * u) @ w_down[l]
    return rms_norm(x, final_g)
```

```python
import numpy as np
import ml_dtypes
import concourse.bass as bass
import concourse.mybir as mybir
from concourse.bass_utils import run_bass_kernel_spmd

F32 = mybir.dt.float32
BF16 = mybir.dt.bfloat16
I32 = mybir.dt.int32
ALU = mybir.AluOpType
AF = mybir.ActivationFunctionType

D = 2048
S = 2048
DEPTH = 2
KC = 16
T = 512
NT = S // T
NB = T // 128
DFF = 5632
NFF = DFF // 128
NG = 2
GFF = NFF // NG
EPS = 1e-6
SLOT = 4096
NCORE = 8


class Buf:
    __slots__ = ("ap", "name", "w", "r", "psum")

    def __init__(self, ap, name=""):
        self.ap = ap
        self.name = name
        self.w = None
        self.r = []
        self.psum = False

    def __getitem__(self, idx):
        return V(self, self.ap[idx])


class V:
    __slots__ = ("buf", "ap")

    def __init__(self, buf, ap):
        self.buf = buf
        self.ap = ap

    def __getitem__(self, idx):
        return V(self.buf, self.ap[idx])

    def re(self, pat, **kw):
        return V(self.buf, self.ap.rearrange(pat, **kw))


def _ap(x):
    return x.ap if isinstance(x, (V, Buf)) else x


def _b(x):
    if isinstance(x, V):
        return x.buf
    if isinstance(x, Buf):
        return x
    return None


class FW:
    def __init__(self, nc):
        self.nc = nc
        self.eng = {"pe": nc.tensor, "act": nc.scalar, "dve": nc.vector,
                    "pool": nc.gpsimd, "sp": nc.sync}
        self.semobj = {k: nc.alloc_semaphore("s_" + k) for k in self.eng}
        self.cnt = {k: 0 for k in self.eng}
        self.obs = {k: {} for k in self.eng}
        self.prog = {k: [] for k in self.eng}
        self.pend = []
        self.n_inst = 0
        self.n_wait = 0

    def sb(self, name, shape, dt):
        return Buf(self.nc.alloc_sbuf_tensor(name, list(shape), dt).ap(), name)

    def ps(self, name, shape, dt=F32):
        b = Buf(self.nc.alloc_psum_tensor(name, list(shape), dt).ap(), name)
        b.psum = True
        return b

    def new_dma_sem(self, name):
        self.semobj[name] = self.nc.alloc_semaphore("s_" + name)
        self.cnt[name] = 0
        return name

    def _need(self, e, ev):
        if ev is None:
            return
        key, val = ev
        if key == e and e == "pe":
            return
        if self.obs[e].get(key, 0) >= val:
            return
        self.pend.append((self.semobj[key], val))
        self.obs[e][key] = val
        self.n_wait += 1

    def _deps(self, e, reads, writes):
        for v in reads:
            b = _b(v)
            if b is not None:
                self._need(e, b.w)
                if b.psum:
                    for ev in b.r:
                        if ev[0] != e:
                            self._need(e, ev)
        for v in writes:
            b = _b(v)
            if b is not None:
                self._need(e, b.w)
                for ev in b.r:
                    self._need(e, ev)

    def _commit(self, ev, reads, writes):
        for v in reads:
            b = _b(v)
            if b is not None:
                b.r.append(ev)
                if len(b.r) > 32:
                    mx = {}
                    for k, val in b.r:
                        if mx.get(k, 0) < val:
                            mx[k] = val
                    b.r = list(mx.items())
        for v in writes:
            b = _b(v)
            if b is not None:
                b.w = ev
                b.r = []

    def op(self, e, fn, reads, writes):
        self.pend = []
        self._deps(e, reads, writes)
        self.cnt[e] += 1
        self.prog[e].append((self.pend, fn, self.semobj[e], 1))
        self._commit((e, self.cnt[e]), reads, writes)
        self.n_inst += 1

    def mm(self, out, lhsT, rhs, start=True, stop=True):
        o, l, r = _ap(out), _ap(lhsT), _ap(rhs)
        self.op("pe", lambda: self.nc.tensor.matmul(o, l, r, start=start, stop=stop),
                [lhsT, rhs], [out])

    def act(self, out, in_, func, bias=None, scale=None):
        kw = {}
        rd = [in_]
        if bias is not None:
            kw["bias"] = _ap(bias)
            rd.append(bias)
        if scale is not None:
            kw["scale"] = _ap(scale)
            rd.append(scale)
        o, i = _ap(out), _ap(in_)
        self.op("act", lambda: self.nc.scalar.activation(o, i, func, **kw), rd, [out])

    def tt(self, e, out, in0, in1, op):
        o, a, b = _ap(out), _ap(in0), _ap(in1)
        self.op(e, lambda: self.eng[e].tensor_tensor(o, a, b, op), [in0, in1], [out])

    def ts(self, e, out, in0, s1, s2, op0, op1=None):
        kw = {}
        if op1 is not None:
            kw["op1"] = op1
        o, a, x1 = _ap(out), _ap(in0), _ap(s1)
        x2 = _ap(s2) if s2 is not None else None
        self.op(e, lambda: self.eng[e].tensor_scalar(o, a, x1, x2, op0, **kw),
                [in0, s1, s2], [out])

    def stt(self, e, out, in0, scalar, in1, op0, op1):
        o, a, s, b = _ap(out), _ap(in0), _ap(scalar), _ap(in1)
        self.op(e, lambda: self.eng[e].scalar_tensor_tensor(o, a, s, b, op0, op1),
                [in0, scalar, in1], [out])

    def copy(self, e, out, in_):
        o, i = _ap(out), _ap(in_)
        if e == "act":
            self.op(e, lambda: self.nc.scalar.copy(o, i), [in_], [out])
        else:
            self.op(e, lambda: self.eng[e].tensor_copy(o, i), [in_], [out])

    def memset(self, e, out, val):
        o = _ap(out)
        self.op(e, lambda: self.eng[e].memset(o, val), [], [out])

    def recip(self, out, in_):
        o, i = _ap(out), _ap(in_)
        self.op("dve", lambda: self.nc.vector.reciprocal(o, i), [in_], [out])

    def dma(self, q, out, in_, semkey):
        self.pend = []
        self._deps(q, [in_], [out])
        o, i = _ap(out), _ap(in_)
        self.cnt[semkey] += 16
        self.prog[q].append((self.pend, lambda: self.eng[q].dma_start(out=o, in_=i),
                             self.semobj[semkey], 16))
        self._commit((semkey, self.cnt[semkey]), [in_], [out])
        self.n_inst += 1

    def dma_multi(self, q, pairs, semkey):
        self.pend = []
        for out, in_ in pairs:
            self._deps(q, [in_], [out])
        first = True
        final = self.cnt[semkey] + 16 * len(pairs)
        for out, in_ in pairs:
            o, i = _ap(out), _ap(in_)
            self.prog[q].append((self.pend if first else [],
                                 (lambda o=o, i=i: self.eng[q].dma_start(out=o, in_=i)),
                                 self.semobj[semkey], 16))
            first = False
            self.n_inst += 1
        self.cnt[semkey] = final
        for out, in_ in pairs:
            self._commit((semkey, final), [in_], [out])

    def wait_all(self, e, bufs):
        self.pend = []
        for b in bufs:
            b = _b(b)
            self._need(e, b.w)
            for ev in b.r:
                self._need(e, ev)
        self.prog[e].append((self.pend, None, None, 0))

    def finalize(self):
        nc = self.nc
        with nc.Block() as block:
            def run(e):
                def body(engine):
                    for waits, fn, sem, inc in self.prog[e]:
                        for s_, v_ in waits:
                            engine.wait_ge(s_, v_)
                        if fn is not None:
                            fn().then_inc(sem, inc)
                return body
            block.sync(run("sp"))
            block.tensor(run("pe"))
            block.scalar(run("act"))
            block.vector(run("dve"))
            block.gpsimd(run("pool"))


class Ring:
    def __init__(self, bufs):
        self.bufs = bufs
        self.i = 0

    def next(self):
        b = self.bufs[self.i % len(self.bufs)]
        self.i += 1
        return b


IN_OFF = dict(qa=0, ka=1024, va=1280, qb=1536, kb=2048, vb=2560, rb=3584,
              al=4608, ga=4624, gb=6672)


def _fm(Wcols):
    n = Wcols.shape[1]
    return np.ascontiguousarray(Wcols.reshape(KC, 128, n).transpose(1, 0, 2))


def pack_weights(w_in, w_branch_a, w_branch_b, w_out, w_up, w_down, depth):
    parts = []
    table = {}
    off = 0

    def add(key, arr):
        nonlocal off
        P = arr.shape[0]
        a2 = np.ascontiguousarray(arr.reshape(P, -1), dtype=np.float32)
        table[key] = (off, P, a2.shape[1])
        parts.append(a2.reshape(-1))
        off += a2.size

    for l in range(depth):
        W = w_in[l]
        add((l, "al"), _fm(W[:, IN_OFF["al"]:IN_OFF["al"] + 16]))
        add((l, "k"), _fm(W[:, IN_OFF["ka"]:IN_OFF["ka"] + 256]))
        add((l, "v"), _fm(W[:, IN_OFF["va"]:IN_OFF["va"] + 256]))
        for g in range(4):
            add((l, "qa", g), _fm(W[:, IN_OFF["qa"] + g * 256:IN_OFF["qa"] + (g + 1) * 256]))
        for h in range(4):
            qk = np.concatenate([W[:, IN_OFF["qb"] + h * 128:IN_OFF["qb"] + (h + 1) * 128],
                                 W[:, IN_OFF["kb"] + h * 128:IN_OFF["kb"] + (h + 1) * 128]], 1)
            add((l, "qkb", h), _fm(qk))
            add((l, "vb", h), _fm(W[:, IN_OFF["vb"] + h * 256:IN_OFF["vb"] + (h + 1) * 256]))
            add((l, "rb", h), _fm(W[:, IN_OFF["rb"] + h * 256:IN_OFF["rb"] + (h + 1) * 256]))
        for n in range(16):
            gg = np.concatenate([W[:, IN_OFF["ga"] + n * 128:IN_OFF["ga"] + (n + 1) * 128],
                                 W[:, IN_OFF["gb"] + n * 128:IN_OFF["gb"] + (n + 1) * 128]], 1)
            add((l, "gate", n), _fm(gg))
            wa = w_branch_a[l][:, n * 128:(n + 1) * 128].reshape(16, 64, 128).transpose(1, 0, 2)
            add((l, "wa", n), wa)
            wb = w_branch_b[l][:, n * 128:(n + 1) * 128].reshape(8, 128, 128).transpose(1, 0, 2)
            add((l, "wb", n), wb)
        for n2 in range(16):
            add((l, "wo", n2), _fm(w_out[l][:, n2 * 128:(n2 + 1) * 128]))
        for i in range(NFF):
            up = np.concatenate([w_up[l][:, i * 128:(i + 1) * 128],
                                 w_up[l][:, DFF + i * 128:DFF + (i + 1) * 128]], 1)
            add((l, "up", i), _fm(up))
        for gi in range(NG):
            for n2 in range(16):
                wd = w_down[l][gi * GFF * 128:(gi + 1) * GFF * 128, n2 * 128:(n2 + 1) * 128]
                add((l, "dn", gi, n2), wd.reshape(GFF, 128, 128).transpose(1, 0, 2))
    return np.concatenate(parts), table


def slab_table(depth):
    table = {}
    off = 0

    def add(key, P, E):
        nonlocal off
        table[key] = (off, P, E)
        off += P * E
    for l in range(depth):
        add((l, "al"), 128, 16 * 16)
        add((l, "k"), 128, 16 * 256)
        add((l, "v"), 128, 16 * 256)
        for g in range(4):
            add((l, "qa", g), 128, 16 * 256)
        for h in range(4):
            add((l, "qkb", h), 128, 16 * 256)
            add((l, "vb", h), 128, 16 * 256)
            add((l, "rb", h), 128, 16 * 256)
        for n in range(16):
            add((l, "gate", n), 128, 16 * 256)
            add((l, "wa", n), 64, 16 * 128)
            add((l, "wb", n), 128, 8 * 128)
        for n2 in range(16):
            add((l, "wo", n2), 128, 16 * 128)
        for i in range(NFF):
            add((l, "up", i), 128, 16 * 256)
        for gi in range(NG):
            for n2 in range(16):
                add((l, "dn", gi, n2), 128, GFF * 128)
    return table, off


SM_GMIX = 0
SM_GFFN = 32
SM_GFIN = 64
SM_GLAG = 80
SM_CONV = 84
SM_INVF = SM_CONV + 2 * 352
SM_SINK = SM_INVF + 1
SM_RM = SM_SINK + 32
SM_COLS = SM_RM + 2

C_ID = 0
C_U = 128
C_L = 256
C_R = 384
C_MP = 448
C_MC = 960
C_ONE = 1472
CST_COLS = 1600
NEG = -240000.0


def make_consts():
    c = np.zeros((128, CST_COLS), np.float32)
    c[:, C_ID:C_ID + 128] = np.eye(128)
    s_ = np.arange(128)[:, None]
    t_ = np.arange(128)[None, :]
    same = (s_ // 64) == (t_ // 64)
    c[:, C_U:C_U + 128] = (same & (s_ <= t_))
    c[:, C_L:C_L + 128] = (same & (s_ > t_))
    R = np.zeros((64, 64), np.float32)
    for m in range(8):
        R[m + 8, m] = -1.0
        R[m, m + 8] = 1.0
    c[0:64, C_R:C_R + 64] = R
    k_ = np.arange(128)[:, None]
    q_ = np.arange(128)[None, :]
    mprev = np.where(k_ > q_, 0.0, NEG)
    mcur = np.where(q_ >= k_, 0.0, NEG)
    c[:, C_MP:C_MP + 512] = np.tile(mprev, (1, 4))
    c[:, C_MC:C_MC + 512] = np.tile(mcur, (1, 4))
    c[:, C_ONE:C_ONE + 128] = 1.0
    return c.astype(ml_dtypes.bfloat16)


def make_sm(norm_mix_g, norm_ffn_g, final_g, gla_norm_g, conv_w, conv_b, sink, depth):
    sm = np.zeros((128, SM_COLS), np.float32)
    for l in range(depth):
        sm[:, SM_GMIX + l * 16:SM_GMIX + (l + 1) * 16] = norm_mix_g[l].reshape(16, 128).T
        sm[:, SM_GFFN + l * 16:SM_GFFN + (l + 1) * 16] = norm_ffn_g[l].reshape(16, 128).T
        sm[:, SM_GLAG + l * 2:SM_GLAG + (l + 1) * 2] = gla_norm_g[l].reshape(2, 128).T
        cw = np.concatenate([conv_w[l], conv_b[l][None, :]], 0)
        sm[:, SM_CONV + l * 352:SM_CONV + (l + 1) * 352] = \
            cw.reshape(4, 88, 128).transpose(2, 1, 0).reshape(128, 352)
        sm[:, SM_SINK + l * 16:SM_SINK + (l + 1) * 16] = sink[l][None, :]
    sm[:, SM_GFIN:SM_GFIN + 16] = final_g.reshape(16, 128).T
    invf = (np.float32(500000.0) ** (-np.arange(8, dtype=np.float32) * np.float32(2.0) / np.float32(16))).astype(np.float32)
    for p in range(16):
        sm[p, SM_INVF] = invf[p % 8]
    sm[0:64, SM_RM] = 1.0
    sm[64:128, SM_RM + 1] = 1.0
    return sm


class _Stop(Exception):
    pass


def build(depth=DEPTH, ntiles=NT, taps=None, kstop=0):
    taps = taps or []

    def phase(k):
        if kstop and k >= kstop:
            raise _Stop()
    nc = bass.Bass("TRN2", target_bir_lowering=False)
    table, wtot = slab_table(depth)
    xT_d = nc.dram_tensor("xT", [KC, 128, S], F32, kind="ExternalInput").ap()
    pos_d = nc.dram_tensor("posr", [128, S], I32, kind="ExternalInput").ap()
    w_d = nc.dram_tensor("wpack", [wtot], F32, kind="ExternalInput").ap()
    sm_d = nc.dram_tensor("sm", [128, SM_COLS], F32, kind="ExternalInput").ap()
    cst_d = nc.dram_tensor("cst", [128, CST_COLS], BF16, kind="ExternalInput").ap()
    wau_d = nc.dram_tensor("wau", [32, depth * 512], F32, kind="ExternalInput").ap()
    yT_d = nc.dram_tensor("yT", [KC, 128, S], F32, kind="ExternalOutput").ap()
    tap_d = {}
    fw = FW(nc)

    xT = [fw.sb(f"xT{k}", [128, T], F32) for k in range(KC)]
    xn = [fw.sb(f"xn{k}", [128, T], BF16) for k in range(KC)]
    NW = 4
    slots = [fw.sb(f"slot{i}", [128, SLOT], BF16) for i in range(NW)]
    slot_sem = [fw.new_dma_sem(f"w{i}") for i in range(NW)]
    sm = fw.sb("sm_s", [128, SM_COLS], F32)
    cst = fw.sb("cst_s", [128, CST_COLS], BF16)
    wau = fw.sb("wau_s", [32, depth * 512], BF16)
    es = fw.sb("es", [128, 32], F32)
    gs = fw.sb("gs", [128, 80], F32)
    Ct = fw.sb("Ct", [64, T], F32)
    St = fw.sb("St", [64, T], F32)
    posi = fw.sb("posi", [64, T], I32)
    kTall = fw.sb("kTall", [64, 4, 128 + T], BF16)
    vA = [fw.sb(f"vA{i}", [128, 256], BF16) for i in range(NB + 1)]
    qTg = Ring([fw.sb(f"qTg{i}", [64, 4, T], BF16) for i in range(2)])
    ya = [fw.sb(f"ya{g}", [64, 4, T], BF16) for g in range(4)]
    yb = [fw.sb(f"yb{i}", [128, T], BF16) for i in range(8)]
    mixed = [fw.sb(f"mixed{i}", [128, T], BF16) for i in range(16)]
    aT = mixed + yb[0:GFF - 16]
    Fp = Ring([fw.sb(f"F{i}", [128, T], F32) for i in range(6)])
    Bp = Ring([fw.sb(f"B{i}", [128, T], BF16) for i in range(4)])
    Hp = Ring([fw.sb(f"H{i}", [128, T + 2], F32) for i in range(2)])
    eqk = [fw.sb(f"eqk{i}", [128, T], F32) for i in range(2)]
    alphaT = fw.sb("alphaT", [32, T], BF16)
    spb = [fw.sb(f"spb{i}", [128, 512], BF16) for i in range(NB)]
    eke = [fw.sb(f"eke{i}", [128, 512], BF16) for i in range(NB)]
    qdec = fw.sb("qdec", [128, T], BF16)
    kinv = fw.sb("kinv", [128, T], BF16)
    kTb = fw.sb("kTb", [128, T], BF16)
    vB = [fw.sb(f"vB{i}", [128, 256], BF16) for i in range(NB)]
    kend = Ring([fw.sb(f"kend{i}", [128, 128], BF16) for i in range(2)])
    attn = Ring([fw.sb(f"attn{i}", [128, 128], BF16) for i in range(2)])
    Sf = [[fw.sb(f"Sf{l}_{h}", [128, 256], F32) for h in range(4)] for l in range(depth)]
    Sb = [Ring([fw.sb(f"Sb{h}_{i}", [128, 256], BF16) for i in range(3)]) for h in range(4)]
    Sb_cur = [None] * 4
    carry = [fw.sb(f"carry{l}", [128, 88, 2], F32) for l in range(depth)]
    kcar = [fw.sb(f"kcar{l}", [64, 4, 128], BF16) for l in range(depth)]
    vcar = [fw.sb(f"vcar{l}", [128, 256], BF16) for l in range(depth)]
    PS = Ring([fw.ps(f"ps{i}", [128, 512], F32) for i in range(6)])
    ops = [fw.ps(f"ops{i}", [128, 512], F32) for i in range(2)]
    ssacc = ops[1]
    print("sbuf bytes remaining / partition:", nc.sbuf_bytes_remaining)

    ident = cst[:, C_ID:C_ID + 128]
    Umat = cst[:, C_U:C_U + 128]
    Lmat = cst[:, C_L:C_L + 128]
    Rmat = cst[0:64, C_R:C_R + 64]
    mprev = cst[:, C_MP:C_MP + 512]
    mcur = cst[:, C_MC:C_MC + 512]
    ones = cst[:, C_ONE:C_ONE + 128]

    seq = []
    for ti in range(ntiles):
        for l in range(depth):
            seq.append((l, "al"))
            seq.append((l, "k"))
            seq.append((l, "v"))
            for g in range(4):
                seq.append((l, "qa", g))
            for h in range(4):
                seq += [(l, "qkb", h), (l, "vb", h), (l, "rb", h)]
            for n in range(16):
                seq += [(l, "gate", n), (l, "wa", n), (l, "wb", n)]
            for n2 in range(16):
                seq.append((l, "wo", n2))
            for gi in range(NG):
                for j in range(GFF):
                    seq.append((l, "up", gi * GFF + j))
                for n2 in range(16):
                    seq.append((l, "dn", gi, n2))
    state = {"issued": 0, "used": 0}

    def prefetch(upto):
        while state["issued"] <= upto and state["issued"] < len(seq):
            i = state["issued"]
            off, P, E = table[seq[i]]
            src = w_d[off:off + P * E].rearrange("(p e) -> p e", p=P)
            fw.dma("pool", slots[i % NW][0:P, 0:E], src, slot_sem[i % NW])
            state["issued"] += 1

    def next_slab(key):
        i = state["used"]
        assert seq[i] == key, (seq[i], key)
        prefetch(i + NW - 1)
        state["used"] += 1
        return slots[i % NW]

    tapbufs = []

    def tap(name, v, shape, dt):
        if name not in taps or name in tap_d:
            return
        tap_d[name] = nc.dram_tensor("tap_" + name, list(shape), dt, kind="ExternalOutput").ap()
        fw.dma("sp", tap_d[name], v, fw.new_dma_sem("tap_" + name))
        tapbufs.append(v)

    ldsem = fw.new_dma_sem("ld")
    fw.dma_multi("sp", [(sm, sm_d), (cst, cst_d)], ldsem)
    wausem = fw.new_dma_sem("wau")
    fw.dma("pool", wau, wau_d, wausem)
    prefetch(NW - 1)
    fw.act(es, sm[:, SM_SINK:SM_SINK + 32], AF.Exp)
    fw.ts("dve", gs, sm[:, 0:80], float(np.sqrt(D)), None, ALU.mult)
    for l in range(depth):
        for h in range(4):
            fw.memset("dve", Sf[l][h], 0.0)
        fw.memset("dve", carry[l], 0.0)
    fw.memset("dve", alphaT, 1.0)
    fw.memset("dve", kTall, 0.0)
    for l in range(depth):
        fw.memset("dve", kcar[l], 0.0)
        fw.memset("dve", vcar[l], 0.0)
    for h in range(4):
        b = Sb[h].next()
        fw.memset("dve", b, 0.0)
        Sb_cur[h] = b

    xsem = fw.new_dma_sem("x")
    osem = [fw.new_dma_sem(f"o{i}") for i in range(len(Fp.bufs))]
    possem = fw.new_dma_sem("pos")
    C1 = 6.28125
    PI_LO = 3.1415925
    C2 = float(2 * np.pi - 6.28125)

    def ss_add(kc):
        sq = Bp.next()
        fw.act(sq, xT[kc], AF.Square)
        fw.mm(ssacc, ones, sq, start=(kc == 0), stop=(kc == KC - 1))

    def rms_to_xn(gcol, have_ss=False):
        if not have_ss:
            for kc in range(KC):
                ss_add(kc)
        t = Fp.next()
        fw.act(t, ssacc, AF.Sqrt, bias=float(D * EPS), scale=1.0)
        rstd = Fp.next()
        fw.recip(rstd, t)
        return rstd

    def rope_store(z_ps, dst):
        zb = Bp.next()
        fw.copy("act", zb[0:64, :], z_ps)
        phase(3.41)
        sw = PS.next()
        fw.mm(sw[0:64, :], Rmat, zb[0:64, :])
        phase(3.42)
        t1 = Fp.next()
        fw.tt("dve", t1[0:64, :], z_ps, Ct, ALU.mult)
        phase(3.43)
        t2 = Fp.next()
        fw.tt("dve", t2[0:64, :], sw[0:64, :], St, ALU.mult)
        phase(3.44)
        fw.tt("dve", dst, t1[0:64, :], t2[0:64, :], ALU.add)

    try:
      for ti in range(ntiles):
          t0 = ti * T
          fw.dma_multi("sp", [(xT[k], xT_d[k, :, t0:t0 + T]) for k in range(KC)], xsem)
          fw.dma("sp", posi, pos_d[0:64, t0:t0 + T], possem)
          posf = Fp.next()
          fw.copy("dve", posf[0:64, :], posi)
          ang = Fp.next()
          fw.ts("dve", ang[0:64, :], posf[0:64, :], sm[0:64, SM_INVF:SM_INVF + 1], None, ALU.mult)
          kq = Fp.next()
          fw.ts("dve", kq[0:64, :], ang[0:64, :], float(1.0 / (2 * np.pi)), None, ALU.mult)
          fw.copy("dve", posi, kq[0:64, :])
          fw.copy("dve", kq[0:64, :], posi)
          r1 = Fp.next()
          fw.stt("dve", r1[0:64, :], kq[0:64, :], -C1, ang[0:64, :], ALU.mult, ALU.add)
          fw.stt("dve", r1[0:64, :], kq[0:64, :], -C2, r1[0:64, :], ALU.mult, ALU.add)
          fw.ts("dve", r1[0:64, :], r1[0:64, :], -PI_LO, PI_LO, ALU.max, ALU.min)
          fw.act(St, r1[0:64, :], AF.Sin)
          ra = Fp.next()
          fw.act(ra[0:64, :], r1[0:64, :], AF.Sin, scale=0.5)
          fw.tt("dve", ra[0:64, :], ra[0:64, :], ra[0:64, :], ALU.mult)
          fw.ts("dve", Ct, ra[0:64, :], -2.0, 1.0, ALU.mult, ALU.add)

          phase(1)
          for l in range(depth):
              rs = rms_to_xn(None, have_ss=(l > 0))
              for kc in range(KC):
                  fw.stt("dve", xn[kc], xT[kc], gs[:, SM_GMIX + l * 16 + kc:SM_GMIX + l * 16 + kc + 1],
                         rs, ALU.mult, ALU.mult)
              if ti == 0 and l == 0:
                  tap("xn", xn[3], [128, T], BF16)
              sl = next_slab((l, "al"))
              w = sl[:, 0:256].re("p (k c) -> p k c", c=16)
              ap_ = PS.next()
              for kc in range(KC):
                  fw.mm(ap_[0:16, :], w[:, kc, :], xn[kc], start=(kc == 0), stop=(kc == KC - 1))
              fw.copy("act", alphaT[0:16, :], ap_[0:16, :])
              def alpha_u(bi):
                  up_ = PS.next()
                  fw.mm(up_, alphaT[:, bi * 128:(bi + 1) * 128], wau[:, l * 512:(l + 1) * 512])
                  e1 = Fp.next()
                  fw.act(e1, up_, AF.Exp, scale=-1.0)
                  fw.act(spb[bi], e1, AF.Ln, bias=1.0)

              def alpha_l(bi):
                  er = PS.next()
                  fw.mm(er, Lmat, spb[bi])
                  fw.act(eke[bi], er, AF.Exp, scale=-1.0 / 16.0)
              phase(3)
              for g in range(4):
                  fw.copy("act", kTall[:, g, 0:128], kcar[l][:, g, :])
              fw.copy("act", vA[0], vcar[l])
              sl = next_slab((l, "k"))
              w = sl[:, 0:4096].re("p (k c) -> p k c", c=256)
              for g in range(4):
                  kp = PS.next()
                  for kc in range(KC):
                      fw.mm(kp[0:64, :], w[:, kc, g * 64:(g + 1) * 64], xn[kc],
                            start=(kc == 0), stop=(kc == KC - 1))
                  alpha_u(g)
                  if g > 0:
                      alpha_l(g - 1)
                  rope_store(kp[0:64, :], kTall[:, g, 128:128 + T])
              alpha_l(NB - 1)
              sl = next_slab((l, "v"))
              w = sl[:, 0:4096].re("p (k c) -> p k c", c=256)
              for bi in range(NB):
                  vp = PS.next()
                  for kc in range(KC):
                      fw.mm(vp[:, 0:256], xn[kc][:, bi * 128:(bi + 1) * 128], w[:, kc, :],
                            start=(kc == 0), stop=(kc == KC - 1))
                  fw.copy("act", vA[bi + 1], vp[:, 0:256])
              def q_head(g, hh, qt, w):
                  qp = PS.next()
                  for kc in range(KC):
                      fw.mm(qp[0:64, :], w[:, kc, hh * 64:(hh + 1) * 64], xn[kc],
                            start=(kc == 0), stop=(kc == KC - 1))
                  rope_store(qp[0:64, :], qt[:, hh, :])

              def attn_scores(g, bi, qt):
                  first_blk = (ti == 0 and bi == 0)
                  js = [1] if first_blk else [0, 1]
                  sps = []
                  for j in js:
                      kcol = bi * 128 + j * 128
                      sp_ = PS.next()
                      fw.mm(sp_, kTall[:, g, kcol:kcol + 128], qt[:, :, bi * 128:(bi + 1) * 128],
                            start=True, stop=False)
                      fw.mm(sp_, ident, mprev if j == 0 else mcur, start=False, stop=True)
                      sps.append((j, sp_))
                  return sps

              def attn_finish(g, bi, sps):
                  op_ = PS.next()
                  dp_ = PS.next()
                  pjs = []
                  for j, sp_ in sps:
                      pj = Bp.next()
                      fw.act(pj, sp_, AF.Exp, scale=0.125)
                      pjs.append((j, pj))
                  for jn, (j, pj) in enumerate(pjs):
                      fw.mm(op_[0:64, :], vA[bi + j][:, g * 64:(g + 1) * 64], pj,
                            start=(jn == 0), stop=(jn == len(pjs) - 1))
                      fw.mm(dp_[0:64, :], ones[:, 0:64], pj,
                            start=(jn == 0), stop=(jn == len(pjs) - 1))
                  rd = Fp.next()
                  for hh in range(4):
                      c0 = l * 16 + g * 4 + hh
                      fw.ts("dve", rd[0:64, hh * 128:(hh + 1) * 128], dp_[0:64, hh * 128:(hh + 1) * 128],
                            es[0:64, c0:c0 + 1], None, ALU.add)
                  rr = Fp.next()
                  fw.recip(rr[0:64, :], rd[0:64, :])
                  fw.tt("dve", ya[g][:, :, bi * 128:(bi + 1) * 128],
                        op_[0:64, :].re("p (h t) -> p h t", h=4),
                        rr[0:64, :].re("p (h t) -> p h t", h=4), ALU.mult)

              phase(4)
              sl = next_slab((l, "qa", 0))
              wq = sl[:, 0:4096].re("p (k c) -> p k c", c=256)
              qt_cur = qTg.next()
              for hh in range(4):
                  q_head(0, hh, qt_cur, wq)
              if ti == 0 and l == 0:
                  tap("qT", qt_cur, [64, 4, T], BF16)
              for g in range(4):
                  if g == 1:
                      phase(5)
                  if g < 3:
                      sl = next_slab((l, "qa", g + 1))
                      wq = sl[:, 0:4096].re("p (k c) -> p k c", c=256)
                      qt_nxt = qTg.next()
                  for bi in range(NB):
                      sps = attn_scores(g, bi, qt_cur)
                      if g < 3:
                          q_head(g + 1, bi, qt_nxt, wq)
                      attn_finish(g, bi, sps)
                  if g < 3:
                      qt_cur = qt_nxt
              if ti == 0 and l == 0:
                  tap("ya0", ya[0], [64, 4, T], BF16)
              for g in range(4):
                  fw.copy("act", kcar[l][:, g, :], kTall[:, g, T:T + 128])
              fw.copy("act", vcar[l], vA[NB])
              for h in range(4):
                  csp = PS.next()
                  for bi in range(NB):
                      fw.mm(csp[:, bi * 128:(bi + 1) * 128], spb[bi][:, h * 128:(h + 1) * 128], Umat)
                  eq, ek = eqk
                  fw.act(eq, csp, AF.Exp, scale=-1.0 / 16.0)
                  fw.act(ek, csp, AF.Exp, scale=1.0 / 16.0)
                  sl = next_slab((l, "qkb", h))
                  w = sl[:, 0:4096].re("p (k c) -> p k c", c=256)
                  qp = PS.next()
                  for kc in range(KC):
                      fw.mm(qp, w[:, kc, 0:128], xn[kc], start=(kc == 0), stop=(kc == KC - 1))
                  fw.stt("dve", qdec, qp, float(128 ** -0.5), eq, ALU.mult, ALU.mult)
                  kp = PS.next()
                  for kc in range(KC):
                      fw.mm(kp, w[:, kc, 128:256], xn[kc], start=(kc == 0), stop=(kc == KC - 1))
                  fw.tt("dve", kinv, kp, ek, ALU.mult)
                  fw.copy("act", kTb, kp)
                  sl = next_slab((l, "vb", h))
                  w = sl[:, 0:4096].re("p (k c) -> p k c", c=256)
                  for bi in range(NB):
                      vp = PS.next()
                      for kc in range(KC):
                          fw.mm(vp[:, 0:256], xn[kc][:, bi * 128:(bi + 1) * 128], w[:, kc, :],
                                start=(kc == 0), stop=(kc == KC - 1))
                      fw.copy("act", vB[bi], vp[:, 0:256])
                  for bi in range(NB):
                      blk = slice(bi * 128, (bi + 1) * 128)
                      ktp = PS.next()
                      fw.mm(ktp[:, 0:128], kTb[:, blk], ident)
                      kes = []
                      for ci in range(2):
                          ke = kend.next()
                          fw.stt("dve", ke, ktp[:, 0:128], sm[:, SM_RM + ci:SM_RM + ci + 1],
                                 eke[bi][:, h * 128:(h + 1) * 128], ALU.mult, ALU.mult)
                          kes.append(ke)
                      atp = PS.next()
                      fw.mm(atp[:, 0:128], kinv[:, blk], qdec[:, blk])
                      at = attn.next()
                      fw.tt("dve", at, atp[:, 0:128], Umat, ALU.mult)
                      dsp = PS.next()
                      for ci in range(2):
                          fw.mm(dsp[:, ci * 256:(ci + 1) * 256], kes[ci], vB[bi])
                      sb_in = []
                      for ci in range(2):
                          c0 = bi * 128 + ci * 64
                          sb_in.append(Sb_cur[h])
                          fw.stt("dve", Sf[l][h], Sf[l][h], eq[:, c0 + 63:c0 + 64],
                                 dsp[:, ci * 256:(ci + 1) * 256], ALU.mult, ALU.add)
                          nb_ = Sb[h].next()
                          fw.copy("act", nb_, Sf[l][h])
                          Sb_cur[h] = nb_
                      for ci in range(2):
                          c0 = bi * 128 + ci * 64
                          for e2 in range(2):
                              fw.mm(ops[e2][:, c0:c0 + 64], vB[bi][:, e2 * 128:(e2 + 1) * 128],
                                    at[:, ci * 64:(ci + 1) * 64], start=True, stop=False)
                              fw.mm(ops[e2][:, c0:c0 + 64], sb_in[ci][:, e2 * 128:(e2 + 1) * 128],
                                    qdec[:, c0:c0 + 64], start=False, stop=True)
                  ssn = PS.next()
                  for e2 in range(2):
                      sq = Bp.next()
                      fw.act(sq, ops[e2], AF.Square)
                      fw.mm(ssn, ones, sq, start=(e2 == 0), stop=(e2 == 1))
                  tq = Fp.next()
                  fw.act(tq, ssn, AF.Sqrt, bias=float(EPS), scale=1.0 / 256.0)
                  rg = Fp.next()
                  fw.recip(rg, tq)
                  sl = next_slab((l, "rb", h))
                  w = sl[:, 0:4096].re("p (k c) -> p k c", c=256)
                  for e2 in range(2):
                      rp = PS.next()
                      for kc in range(KC):
                          fw.mm(rp, w[:, kc, e2 * 128:(e2 + 1) * 128], xn[kc],
                                start=(kc == 0), stop=(kc == KC - 1))
                      sr = Fp.next()
                      fw.act(sr, rp, AF.Silu)
                      tmp = Fp.next()
                      fw.stt("dve", tmp, ops[e2], sm[:, SM_GLAG + l * 2 + e2:SM_GLAG + l * 2 + e2 + 1],
                             rg, ALU.mult, ALU.mult)
                      fw.tt("dve", yb[h * 2 + e2], tmp, sr, ALU.mult)
              if ti == 0 and l == 0:
                  tap("yb0", yb[0], [128, T], BF16)
              for n in range(16):
                  sl = next_slab((l, "gate", n))
                  w = sl[:, 0:4096].re("p (k c) -> p k c", c=256)
                  gap = PS.next()
                  for kc in range(KC):
                      fw.mm(gap, w[:, kc, 0:128], xn[kc], start=(kc == 0), stop=(kc == KC - 1))
                  sga = Fp.next()
                  fw.act(sga, gap, AF.Sigmoid)
                  gbp = PS.next()
                  for kc in range(KC):
                      fw.mm(gbp, w[:, kc, 128:256], xn[kc], start=(kc == 0), stop=(kc == KC - 1))
                  sgb = Fp.next()
                  fw.act(sgb, gbp, AF.Sigmoid)
                  sl = next_slab((l, "wa", n))
                  w = sl[0:64, 0:2048].re("p (h c) -> p h c", c=128)
                  pap = PS.next()
                  for hh in range(16):
                      fw.mm(pap, w[:, hh, :], ya[hh // 4][:, hh % 4, :], start=(hh == 0), stop=(hh == 15))
                  t1 = Fp.next()
                  fw.tt("dve", t1, sga, pap, ALU.mult)
                  sl = next_slab((l, "wb", n))
                  w = sl[:, 0:1024].re("p (h c) -> p h c", c=128)
                  pbp = PS.next()
                  for ec in range(8):
                      fw.mm(pbp, w[:, ec, :], yb[ec], start=(ec == 0), stop=(ec == 7))
                  t2 = Fp.next()
                  fw.tt("dve", t2, sgb, pbp, ALU.mult)
                  fw.tt("dve", mixed[n], t1, t2, ALU.add)
              if ti == 0 and l == 0:
                  tap("mixed0", mixed[0], [128, T], BF16)
              phase(9)
              for n2 in range(16):
                  sl = next_slab((l, "wo", n2))
                  w = sl[:, 0:2048].re("p (k c) -> p k c", c=128)
                  yp = PS.next()
                  for n in range(16):
                      fw.mm(yp, w[:, n, :], mixed[n], start=(n == 0), stop=(n == 15))
                  fw.tt("dve", xT[n2], xT[n2], yp, ALU.add)
                  if n2 >= 2:
                      ss_add(n2 - 2)
              ss_add(14)
              ss_add(15)
              if ti == 0 and l == 0:
                  tap("xmid", xT[0], [128, T], F32)
              phase(10)
              rs = rms_to_xn(None, have_ss=True)
              for kc in range(KC):
                  fw.stt("dve", xn[kc], xT[kc], gs[:, SM_GFFN + l * 16 + kc:SM_GFFN + l * 16 + kc + 1],
                         rs, ALU.mult, ALU.mult)
              for gi in range(NG):
                  for j in range(GFF):
                      i = gi * GFF + j
                      sl = next_slab((l, "up", i))
                      w = sl[:, 0:4096].re("p (k c) -> p k c", c=256)
                      cv = []
                      for part in range(2):
                          ch = part * NFF + i
                          hp = PS.next()
                          for kc in range(KC):
                              fw.mm(hp, w[:, kc, part * 128:(part + 1) * 128], xn[kc],
                                    start=(kc == 0), stop=(kc == KC - 1))
                          hb = Hp.next()
                          fw.copy("dve", hb[:, 0:2], carry[l][:, ch, :])
                          fw.copy("act", hb[:, 2:T + 2], hp)
                          fw.copy("dve", carry[l][:, ch, :], hb[:, T:T + 2])
                          cb = SM_CONV + l * 352 + ch * 4
                          acc = Fp.next()
                          fw.ts("dve", acc, hb[:, 2:T + 2], sm[:, cb + 2:cb + 3], sm[:, cb + 3:cb + 4],
                                ALU.mult, ALU.add)
                          fw.stt("dve", acc, hb[:, 1:T + 1], sm[:, cb + 1:cb + 2], acc, ALU.mult, ALU.add)
                          fw.stt("dve", acc, hb[:, 0:T], sm[:, cb:cb + 1], acc, ALU.mult, ALU.add)
                          cv.append(acc)
                      sg = Fp.next()
                      fw.act(sg, cv[0], AF.Silu)
                      fw.tt("dve", aT[j], sg, cv[1], ALU.mult)
                      if ti == 0 and l == 0 and i == 0:
                          tap("a0", aT[0], [128, T], BF16)
                  for n2 in range(16):
                      sl = next_slab((l, "dn", gi, n2))
                      w = sl[:, 0:GFF * 128].re("p (j c) -> p j c", c=128)
                      yp = PS.next()
                      for j in range(GFF):
                          fw.mm(yp, w[:, j, :], aT[j], start=(j == 0), stop=(j == GFF - 1))
                      fw.tt("dve", xT[n2], xT[n2], yp, ALU.add)
                      if gi == NG - 1 and n2 >= 2:
                          ss_add(n2 - 2)
                  if gi == NG - 1:
                      ss_add(14)
                      ss_add(15)
              if ti == 0 and l == 0:
                  tap("xout", xT[0], [128, T], F32)
              phase(11)
              if depth > 1:
                  nl = (l + 1) % depth
                  for h in range(4):
                      nb_ = Sb[h].next()
                      fw.copy("act", nb_, Sf[nl][h])
                      Sb_cur[h] = nb_
          rs = rms_to_xn(None, have_ss=True)
          for kc in range(KC):
              o = Fp.next()
              if o is rs:
                  o = Fp.next()
              fw.stt("dve", o, xT[kc], gs[:, SM_GFIN + kc:SM_GFIN + kc + 1], rs, ALU.mult, ALU.mult)
              fw.dma("sp", yT_d[kc, :, t0:t0 + T], o, osem[(Fp.i - 1) % len(Fp.bufs)])
    except _Stop:
        pass
    assert kstop or state["used"] == len(seq)
    fw.wait_all("sp", Fp.bufs + tapbufs)
    print("n_inst", fw.n_inst, "n_wait", fw.n_wait)
    fw.finalize()
    return nc, tap_d


_CACHE = {}


def prepare_inputs(x, positions, norm_mix_g, w_in, sink, w_alpha_up, b_alpha, gla_norm_g,
                   w_branch_a, w_branch_b, w_out, norm_ffn_g, w_up, conv_w, conv_b, w_down,
                   final_g, depth=DEPTH, cores=None):
    cores = list(range(NCORE)) if cores is None else cores
    wpack, _ = pack_weights(w_in, w_branch_a, w_branch_b, w_out, w_up, w_down, depth)
    sm = make_sm(norm_mix_g, norm_ffn_g, final_g, gla_norm_g, conv_w, conv_b, sink, depth)
    cst = make_consts()
    wau = np.zeros((32, depth * 512), np.float32)
    for l in range(depth):
        wau[0:16, l * 512:(l + 1) * 512] = w_alpha_up[l]
        wau[16, l * 512:(l + 1) * 512] = b_alpha[l]
    in_maps = []
    for b in cores:
        xT = np.ascontiguousarray(np.asarray(x[b], np.float32).T.reshape(KC, 128, S))
        posr = np.ascontiguousarray(np.broadcast_to(np.asarray(positions[b], np.int32)[None, :], (128, S)))
        in_maps.append({"xT": xT, "posr": posr, "wpack": wpack, "sm": sm, "cst": cst, "wau": wau})
    return in_maps


def kernel(**inputs):
    inputs = {k: np.asarray(v) for k, v in inputs.items()}
    in_maps = prepare_inputs(**inputs)
    if "nc" not in _CACHE:
        _CACHE["nc"] = build()[0]
    nc = _CACHE["nc"]
    res = run_bass_kernel_spmd(nc, in_maps, core_ids=list(range(NCORE)))
    out = np.empty((NCORE, S, D), np.float32)
    for b in range(NCORE):
        yT = res.results[b]["yT"]
        out[b] = yT.reshape(D, S).T
    return out
```

```python
import numpy as np
import ml_dtypes
import concourse.bass as bass
import concourse.mybir as mybir
from concourse.bass_utils import run_bass_kernel_spmd

F32 = mybir.dt.float32
BF16 = mybir.dt.bfloat16
I32 = mybir.dt.int32
ALU = mybir.AluOpType
AF = mybir.ActivationFunctionType

D = 2048
S = 2048
DEPTH = 2
KC = 16
T = 512
NT = S // T
NB = T // 128
DFF = 5632
NFF = DFF // 128
NG = 2
GFF = NFF // NG
EPS = 1e-6
SLOT = 4096
NCORE = 8


class Buf:
    __slots__ = ("ap", "name", "w", "r", "psum")

    def __init__(self, ap, name=""):
        self.ap = ap
        self.name = name
        self.w = None
        self.r = []
        self.psum = False

    def __getitem__(self, idx):
        return V(self, self.ap[idx])


class V:
    __slots__ = ("buf", "ap")

    def __init__(self, buf, ap):
        self.buf = buf
        self.ap = ap

    def __getitem__(self, idx):
        return V(self.buf, self.ap[idx])

    def re(self, pat, **kw):
        return V(self.buf, self.ap.rearrange(pat, **kw))


def _ap(x):
    return x.ap if isinstance(x, (V, Buf)) else x


def _b(x):
    if isinstance(x, V):
        return x.buf
    if isinstance(x, Buf):
        return x
    return None


class FW:
    def __init__(self, nc):
        self.nc = nc
        self.eng = {"pe": nc.tensor, "act": nc.scalar, "dve": nc.vector,
                    "pool": nc.gpsimd, "sp": nc.sync}
        self.semobj = {k: nc.alloc_semaphore("s_" + k) for k in self.eng}
        self.cnt = {k: 0 for k in self.eng}
        self.obs = {k: {} for k in self.eng}
        self.prog = {k: [] for k in self.eng}
        self.pend = []
        self.n_inst = 0
        self.n_wait = 0

    def sb(self, name, shape, dt):
        return Buf(self.nc.alloc_sbuf_tensor(name, list(shape), dt).ap(), name)

    def ps(self, name, shape, dt=F32):
        b = Buf(self.nc.alloc_psum_tensor(name, list(shape), dt).ap(), name)
        b.psum = True
        return b

    def new_dma_sem(self, name):
        self.semobj[name] = self.nc.alloc_semaphore("s_" + name)
        self.cnt[name] = 0
        return name

    def _need(self, e, ev):
        if ev is None:
            return
        key, val = ev
        if key == e and e == "pe":
            return
        if self.obs[e].get(key, 0) >= val:
            return
        self.pend.append((self.semobj[key], val))
        self.obs[e][key] = val
        self.n_wait += 1

    def _deps(self, e, reads, writes):
        for v in reads:
            b = _b(v)
            if b is not None:
                self._need(e, b.w)
                if b.psum:
                    for ev in b.r:
                        if ev[0] != e:
                            self._need(e, ev)
        for v in writes:
            b = _b(v)
            if b is not None:
                self._need(e, b.w)
                for ev in b.r:
                    self._need(e, ev)

    def _commit(self, ev, reads, writes):
        for v in reads:
            b = _b(v)
            if b is not None:
                b.r.append(ev)
                if len(b.r) > 32:
                    mx = {}
                    for k, val in b.r:
                        if mx.get(k, 0) < val:
                            mx[k] = val
                    b.r = list(mx.items())
        for v in writes:
            b = _b(v)
            if b is not None:
                b.w = ev
                b.r = []

    def op(self, e, fn, reads, writes):
        self.pend = []
        self._deps(e, reads, writes)
        self.cnt[e] += 1
        self.prog[e].append((self.pend, fn, self.semobj[e], 1))
        self._commit((e, self.cnt[e]), reads, writes)
        self.n_inst += 1

    def mm(self, out, lhsT, rhs, start=True, stop=True):
        o, l, r = _ap(out), _ap(lhsT), _ap(rhs)
        self.op("pe", lambda: self.nc.tensor.matmul(o, l, r, start=start, stop=stop),
                [lhsT, rhs], [out])

    def act(self, out, in_, func, bias=None, scale=None):
        kw = {}
        rd = [in_]
        if bias is not None:
            kw["bias"] = _ap(bias)
            rd.append(bias)
        if scale is not None:
            kw["scale"] = _ap(scale)
            rd.append(scale)
        o, i = _ap(out), _ap(in_)
        self.op("act", lambda: self.nc.scalar.activation(o, i, func, **kw), rd, [out])

    def tt(self, e, out, in0, in1, op):
        o, a, b = _ap(out), _ap(in0), _ap(in1)
        self.op(e, lambda: self.eng[e].tensor_tensor(o, a, b, op), [in0, in1], [out])

    def ts(self, e, out, in0, s1, s2, op0, op1=None):
        kw = {}
        if op1 is not None:
            kw["op1"] = op1
        o, a, x1 = _ap(out), _ap(in0), _ap(s1)
        x2 = _ap(s2) if s2 is not None else None
        self.op(e, lambda: self.eng[e].tensor_scalar(o, a, x1, x2, op0, **kw),
                [in0, s1, s2], [out])

    def stt(self, e, out, in0, scalar, in1, op0, op1):
        o, a, s, b = _ap(out), _ap(in0), _ap(scalar), _ap(in1)
        self.op(e, lambda: self.eng[e].scalar_tensor_tensor(o, a, s, b, op0, op1),
                [in0, scalar, in1], [out])

    def copy(self, e, out, in_):
        o, i = _ap(out), _ap(in_)
        if e == "act":
            self.op(e, lambda: self.nc.scalar.copy(o, i), [in_], [out])
        else:
            self.op(e, lambda: self.eng[e].tensor_copy(o, i), [in_], [out])

    def memset(self, e, out, val):
        o = _ap(out)
        self.op(e, lambda: self.eng[e].memset(o, val), [], [out])

    def recip(self, out, in_):
        o, i = _ap(out), _ap(in_)
        self.op("dve", lambda: self.nc.vector.reciprocal(o, i), [in_], [out])

    def dma(self, q, out, in_, semkey):
        self.pend = []
        self._deps(q, [in_], [out])
        o, i = _ap(out), _ap(in_)
        self.cnt[semkey] += 16
        self.prog[q].append((self.pend, lambda: self.eng[q].dma_start(out=o, in_=i),
                             self.semobj[semkey], 16))
        self._commit((semkey, self.cnt[semkey]), [in_], [out])
        self.n_inst += 1

    def dma_multi(self, q, pairs, semkey):
        self.pend = []
        for out, in_ in pairs:
            self._deps(q, [in_], [out])
        first = True
        final = self.cnt[semkey] + 16 * len(pairs)
        for out, in_ in pairs:
            o, i = _ap(out), _ap(in_)
            self.prog[q].append((self.pend if first else [],
                                 (lambda o=o, i=i: self.eng[q].dma_start(out=o, in_=i)),
                                 self.semobj[semkey], 16))
            first = False
            self.n_inst += 1
        self.cnt[semkey] = final
        for out, in_ in pairs:
            self._commit((semkey, final), [in_], [out])

    def wait_all(self, e, bufs):
        self.pend = []
        for b in bufs:
            b = _b(b)
            self._need(e, b.w)
            for ev in b.r:
                self._need(e, ev)
        self.prog[e].append((self.pend, None, None, 0))

    def finalize(self):
        nc = self.nc
        with nc.Block() as block:
            def run(e):
                def body(engine):
                    for waits, fn, sem, inc in self.prog[e]:
                        for s_, v_ in waits:
                            engine.wait_ge(s_, v_)
                        if fn is not None:
                            fn().then_inc(sem, inc)
                return body
            block.sync(run("sp"))
            block.tensor(run("pe"))
            block.scalar(run("act"))
            block.vector(run("dve"))
            block.gpsimd(run("pool"))


class Ring:
    def __init__(self, bufs):
        self.bufs = bufs
        self.i = 0

    def next(self):
        b = self.bufs[self.i % len(self.bufs)]
        self.i += 1
        return b


IN_OFF = dict(qa=0, ka=1024, va=1280, qb=1536, kb=2048, vb=2560, rb=3584,
              al=4608, ga=4624, gb=6672)


def _fm(Wcols):
    n = Wcols.shape[1]
    return np.ascontiguousarray(Wcols.reshape(KC, 128, n).transpose(1, 0, 2))


def pack_weights(w_in, w_branch_a, w_branch_b, w_out, w_up, w_down, depth):
    parts = []
    table = {}
    off = 0

    def add(key, arr):
        nonlocal off
        P = arr.shape[0]
        a2 = np.ascontiguousarray(arr.reshape(P, -1), dtype=np.float32)
        table[key] = (off, P, a2.shape[1])
        parts.append(a2.reshape(-1))
        off += a2.size

    for l in range(depth):
        W = w_in[l]
        add((l, "al"), _fm(W[:, IN_OFF["al"]:IN_OFF["al"] + 16]))
        add((l, "k"), _fm(W[:, IN_OFF["ka"]:IN_OFF["ka"] + 256]))
        add((l, "v"), _fm(W[:, IN_OFF["va"]:IN_OFF["va"] + 256]))
        for g in range(4):
            add((l, "qa", g), _fm(W[:, IN_OFF["qa"] + g * 256:IN_OFF["qa"] + (g + 1) * 256]))
        for h in range(4):
            qk = np.concatenate([W[:, IN_OFF["qb"] + h * 128:IN_OFF["qb"] + (h + 1) * 128],
                                 W[:, IN_OFF["kb"] + h * 128:IN_OFF["kb"] + (h + 1) * 128]], 1)
            add((l, "qkb", h), _fm(qk))
            add((l, "vb", h), _fm(W[:, IN_OFF["vb"] + h * 256:IN_OFF["vb"] + (h + 1) * 256]))
            add((l, "rb", h), _fm(W[:, IN_OFF["rb"] + h * 256:IN_OFF["rb"] + (h + 1) * 256]))
        for n in range(16):
            gg = np.concatenate([W[:, IN_OFF["ga"] + n * 128:IN_OFF["ga"] + (n + 1) * 128],
                                 W[:, IN_OFF["gb"] + n * 128:IN_OFF["gb"] + (n + 1) * 128]], 1)
            add((l, "gate", n), _fm(gg))
            wa = w_branch_a[l][:, n * 128:(n + 1) * 128].reshape(16, 64, 128).transpose(1, 0, 2)
            add((l, "wa", n), wa)
            wb = w_branch_b[l][:, n * 128:(n + 1) * 128].reshape(8, 128, 128).transpose(1, 0, 2)
            add((l, "wb", n), wb)
        for n2 in range(16):
            add((l, "wo", n2), _fm(w_out[l][:, n2 * 128:(n2 + 1) * 128]))
        for i in range(NFF):
            up = np.concatenate([w_up[l][:, i * 128:(i + 1) * 128],
                                 w_up[l][:, DFF + i * 128:DFF + (i + 1) * 128]], 1)
            add((l, "up", i), _fm(up))
        for gi in range(NG):
            for n2 in range(16):
                wd = w_down[l][gi * GFF * 128:(gi + 1) * GFF * 128, n2 * 128:(n2 + 1) * 128]
                add((l, "dn", gi, n2), wd.reshape(GFF, 128, 128).transpose(1, 0, 2))
    return np.concatenate(parts), table


def slab_table(depth):
    table = {}
    off = 0

    def add(key, P, E):
        nonlocal off
        table[key] = (off, P, E)
        off += P * E
    for l in range(depth):
        add((l, "al"), 128, 16 * 16)
        add((l, "k"), 128, 16 * 256)
        add((l, "v"), 128, 16 * 256)
        for g in range(4):
            add((l, "qa", g), 128, 16 * 256)
        for h in range(4):
            add((l, "qkb", h), 128, 16 * 256)
            add((l, "vb", h), 128, 16 * 256)
            add((l, "rb", h), 128, 16 * 256)
        for n in range(16):
            add((l, "gate", n), 128, 16 * 256)
            add((l, "wa", n), 64, 16 * 128)
            add((l, "wb", n), 128, 8 * 128)
        for n2 in range(16):
            add((l, "wo", n2), 128, 16 * 128)
        for i in range(NFF):
            add((l, "up", i), 128, 16 * 256)
        for gi in range(NG):
            for n2 in range(16):
                add((l, "dn", gi, n2), 128, GFF * 128)
    return table, off


SM_GMIX = 0
SM_GFFN = 32
SM_GFIN = 64
SM_GLAG = 80
SM_CONV = 84
SM_INVF = SM_CONV + 2 * 352
SM_SINK = SM_INVF + 1
SM_RM = SM_SINK + 32
SM_COLS = SM_RM + 2

C_ID = 0
C_U = 128
C_L = 256
C_R = 384
C_MP = 448
C_MC = 960
C_ONE = 1472
CST_COLS = 1600
NEG = -240000.0


def make_consts():
    c = np.zeros((128, CST_COLS), np.float32)
    c[:, C_ID:C_ID + 128] = np.eye(128)
    s_ = np.arange(128)[:, None]
    t_ = np.arange(128)[None, :]
    same = (s_ // 64) == (t_ // 64)
    c[:, C_U:C_U + 128] = (same & (s_ <= t_))
    c[:, C_L:C_L + 128] = (same & (s_ > t_))
    R = np.zeros((64, 64), np.float32)
    for m in range(8):
        R[m + 8, m] = -1.0
        R[m, m + 8] = 1.0
    c[0:64, C_R:C_R + 64] = R
    k_ = np.arange(128)[:, None]
    q_ = np.arange(128)[None, :]
    mprev = np.where(k_ > q_, 0.0, NEG)
    mcur = np.where(q_ >= k_, 0.0, NEG)
    c[:, C_MP:C_MP + 512] = np.tile(mprev, (1, 4))
    c[:, C_MC:C_MC + 512] = np.tile(mcur, (1, 4))
    c[:, C_ONE:C_ONE + 128] = 1.0
    return c.astype(ml_dtypes.bfloat16)


def make_sm(norm_mix_g, norm_ffn_g, final_g, gla_norm_g, conv_w, conv_b, sink, depth):
    sm = np.zeros((128, SM_COLS), np.float32)
    for l in range(depth):
        sm[:, SM_GMIX + l * 16:SM_GMIX + (l + 1) * 16] = norm_mix_g[l].reshape(16, 128).T
        sm[:, SM_GFFN + l * 16:SM_GFFN + (l + 1) * 16] = norm_ffn_g[l].reshape(16, 128).T
        sm[:, SM_GLAG + l * 2:SM_GLAG + (l + 1) * 2] = gla_norm_g[l].reshape(2, 128).T
        cw = np.concatenate([conv_w[l], conv_b[l][None, :]], 0)
        sm[:, SM_CONV + l * 352:SM_CONV + (l + 1) * 352] = \
            cw.reshape(4, 88, 128).transpose(2, 1, 0).reshape(128, 352)
        sm[:, SM_SINK + l * 16:SM_SINK + (l + 1) * 16] = sink[l][None, :]
    sm[:, SM_GFIN:SM_GFIN + 16] = final_g.reshape(16, 128).T
    invf = (np.float32(500000.0) ** (-np.arange(8, dtype=np.float32) * np.float32(2.0) / np.float32(16))).astype(np.float32)
    for p in range(16):
        sm[p, SM_INVF] = invf[p % 8]
    sm[0:64, SM_RM] = 1.0
    sm[64:128, SM_RM + 1] = 1.0
    return sm


class _Stop(Exception):
    pass


def build(depth=DEPTH, ntiles=NT, taps=None, kstop=0):
    taps = taps or []

    def phase(k):
        if kstop and k >= kstop:
            raise _Stop()
    nc = bass.Bass("TRN2", target_bir_lowering=False)
    table, wtot = slab_table(depth)
    xT_d = nc.dram_tensor("xT", [KC, 128, S], F32, kind="ExternalInput").ap()
    pos_d = nc.dram_tensor("posr", [128, S], I32, kind="ExternalInput").ap()
    w_d = nc.dram_tensor("wpack", [wtot], F32, kind="ExternalInput").ap()
    sm_d = nc.dram_tensor("sm", [128, SM_COLS], F32, kind="ExternalInput").ap()
    cst_d = nc.dram_tensor("cst", [128, CST_COLS], BF16, kind="ExternalInput").ap()
    wau_d = nc.dram_tensor("wau", [32, depth * 512], F32, kind="ExternalInput").ap()
    yT_d = nc.dram_tensor("yT", [KC, 128, S], F32, kind="ExternalOutput").ap()
    tap_d = {}
    fw = FW(nc)

    xT = [fw.sb(f"xT{k}", [128, T], F32) for k in range(KC)]
    xn = [fw.sb(f"xn{k}", [128, T], BF16) for k in range(KC)]
    NW = 4
    slots = [fw.sb(f"slot{i}", [128, SLOT], BF16) for i in range(NW)]
    slot_sem = [fw.new_dma_sem(f"w{i}") for i in range(NW)]
    sm = fw.sb("sm_s", [128, SM_COLS], F32)
    cst = fw.sb("cst_s", [128, CST_COLS], BF16)
    wau = fw.sb("wau_s", [32, depth * 512], BF16)
    es = fw.sb("es", [128, 32], F32)
    gs = fw.sb("gs", [128, 80], F32)
    Ct = fw.sb("Ct", [64, T], F32)
    St = fw.sb("St", [64, T], F32)
    posi = fw.sb("posi", [64, T], I32)
    kTall = fw.sb("kTall", [64, 4, 128 + T], BF16)
    vA = [fw.sb(f"vA{i}", [128, 256], BF16) for i in range(NB + 1)]
    qTg = Ring([fw.sb(f"qTg{i}", [64, 4, T], BF16) for i in range(2)])
    ya = [fw.sb(f"ya{g}", [64, 4, T], BF16) for g in range(4)]
    yb = [fw.sb(f"yb{i}", [128, T], BF16) for i in range(8)]
    mixed = [fw.sb(f"mixed{i}", [128, T], BF16) for i in range(16)]
    aT = mixed + yb[0:GFF - 16]
    Fp = Ring([fw.sb(f"F{i}", [128, T], F32) for i in range(6)])
    Bp = Ring([fw.sb(f"B{i}", [128, T], BF16) for i in range(4)])
    Hp = Ring([fw.sb(f"H{i}", [128, T + 2], F32) for i in range(2)])
    eqk = [fw.sb(f"eqk{i}", [128, T], F32) for i in range(2)]
    alphaT = fw.sb("alphaT", [32, T], BF16)
    spb = [fw.sb(f"spb{i}", [128, 512], BF16) for i in range(NB)]
    eke = [fw.sb(f"eke{i}", [128, 512], BF16) for i in range(NB)]
    qdec = fw.sb("qdec", [128, T], BF16)
    kinv = fw.sb("kinv", [128, T], BF16)
    kTb = fw.sb("kTb", [128, T], BF16)
    vB = [fw.sb(f"vB{i}", [128, 256], BF16) for i in range(NB)]
    kend = Ring([fw.sb(f"kend{i}", [128, 128], BF16) for i in range(2)])
    attn = Ring([fw.sb(f"attn{i}", [128, 128], BF16) for i in range(2)])
    Sf = [[fw.sb(f"Sf{l}_{h}", [128, 256], F32) for h in range(4)] for l in range(depth)]
    Sb = [Ring([fw.sb(f"Sb{h}_{i}", [128, 256], BF16) for i in range(3)]) for h in range(4)]
    Sb_cur = [None] * 4
    carry = [fw.sb(f"carry{l}", [128, 88, 2], F32) for l in range(depth)]
    kcar = [fw.sb(f"kcar{l}", [64, 4, 128], BF16) for l in range(depth)]
    vcar = [fw.sb(f"vcar{l}", [128, 256], BF16) for l in range(depth)]
    PS = Ring([fw.ps(f"ps{i}", [128, 512], F32) for i in range(6)])
    ops = [fw.ps(f"ops{i}", [128, 512], F32) for i in range(2)]
    ssacc = ops[1]
    print("sbuf bytes remaining / partition:", nc.sbuf_bytes_remaining)

    ident = cst[:, C_ID:C_ID + 128]
    Umat = cst[:, C_U:C_U + 128]
    Lmat = cst[:, C_L:C_L + 128]
    Rmat = cst[0:64, C_R:C_R + 64]
    mprev = cst[:, C_MP:C_MP + 512]
    mcur = cst[:, C_MC:C_MC + 512]
    ones = cst[:, C_ONE:C_ONE + 128]

    seq = []
    for ti in range(ntiles):
        for l in range(depth):
            seq.append((l, "al"))
            seq.append((l, "k"))
            seq.append((l, "v"))
            for g in range(4):
                seq.append((l, "qa", g))
            for h in range(4):
                seq += [(l, "qkb", h), (l, "vb", h), (l, "rb", h)]
            for n in range(16):
                seq += [(l, "gate", n), (l, "wa", n), (l, "wb", n)]
            for n2 in range(16):
                seq.append((l, "wo", n2))
            for gi in range(NG):
                for j in range(GFF):
                    seq.append((l, "up", gi * GFF + j))
                for n2 in range(16):
                    seq.append((l, "dn", gi, n2))
    state = {"issued": 0, "used": 0}

    def prefetch(upto):
        while state["issued"] <= upto and state["issued"] < len(seq):
            i = state["issued"]
            off, P, E = table[seq[i]]
            src = w_d[off:off + P * E].rearrange("(p e) -> p e", p=P)
            fw.dma("pool", slots[i % NW][0:P, 0:E], src, slot_sem[i % NW])
            state["issued"] += 1

    def next_slab(key):
        i = state["used"]
        assert seq[i] == key, (seq[i], key)
        prefetch(i + NW - 1)
        state["used"] += 1
        return slots[i % NW]

    tapbufs = []

    def tap(name, v, shape, dt):
        if name not in taps or name in tap_d:
            return
        tap_d[name] = nc.dram_tensor("tap_" + name, list(shape), dt, kind="ExternalOutput").ap()
        fw.dma("sp", tap_d[name], v, fw.new_dma_sem("tap_" + name))
        tapbufs.append(v)

    ldsem = fw.new_dma_sem("ld")
    fw.dma_multi("sp", [(sm, sm_d), (cst, cst_d)], ldsem)
    wausem = fw.new_dma_sem("wau")
    fw.dma("pool", wau, wau_d, wausem)
    prefetch(NW - 1)
    fw.act(es, sm[:, SM_SINK:SM_SINK + 32], AF.Exp)
    fw.ts("dve", gs, sm[:, 0:80], float(np.sqrt(D)), None, ALU.mult)
    for l in range(depth):
        for h in range(4):
            fw.memset("dve", Sf[l][h], 0.0)
        fw.memset("dve", carry[l], 0.0)
    fw.memset("dve", alphaT, 1.0)
    fw.memset("dve", kTall, 0.0)
    for l in range(depth):
        fw.memset("dve", kcar[l], 0.0)
        fw.memset("dve", vcar[l], 0.0)
    for h in range(4):
        b = Sb[h].next()
        fw.memset("dve", b, 0.0)
        Sb_cur[h] = b

    xsem = fw.new_dma_sem("x")
    osem = [fw.new_dma_sem(f"o{i}") for i in range(len(Fp.bufs))]
    possem = fw.new_dma_sem("pos")
    C1 = 6.28125
    PI_LO = 3.1415925
    C2 = float(2 * np.pi - 6.28125)

    def ss_add(kc):
        sq = Bp.next()
        fw.act(sq, xT[kc], AF.Square)
        fw.mm(ssacc, ones, sq, start=(kc == 0), stop=(kc == KC - 1))

    def rms_to_xn(gcol, have_ss=False):
        if not have_ss:
            for kc in range(KC):
                ss_add(kc)
        t = Fp.next()
        fw.act(t, ssacc, AF.Sqrt, bias=float(D * EPS), scale=1.0)
        rstd = Fp.next()
        fw.recip(rstd, t)
        return rstd

    def rope_store(z_ps, dst):
        zb = Bp.next()
        fw.copy("act", zb[0:64, :], z_ps)
        phase(3.41)
        sw = PS.next()
        fw.mm(sw[0:64, :], Rmat, zb[0:64, :])
        phase(3.42)
        t1 = Fp.next()
        fw.tt("dve", t1[0:64, :], z_ps, Ct, ALU.mult)
        phase(3.43)
        t2 = Fp.next()
        fw.tt("dve", t2[0:64, :], sw[0:64, :], St, ALU.mult)
        phase(3.44)
        fw.tt("dve", dst, t1[0:64, :], t2[0:64, :], ALU.add)

    try:
      for ti in range(ntiles):
          t0 = ti * T
          fw.dma_multi("sp", [(xT[k], xT_d[k, :, t0:t0 + T]) for k in range(KC)], xsem)
          fw.dma("sp", posi, pos_d[0:64, t0:t0 + T], possem)
          posf = Fp.next()
          fw.copy("dve", posf[0:64, :], posi)
          ang = Fp.next()
          fw.ts("dve", ang[0:64, :], posf[0:64, :], sm[0:64, SM_INVF:SM_INVF + 1], None, ALU.mult)
          kq = Fp.next()
          fw.ts("dve", kq[0:64, :], ang[0:64, :], float(1.0 / (2 * np.pi)), None, ALU.mult)
          fw.copy("dve", posi, kq[0:64, :])
          fw.copy("dve", kq[0:64, :], posi)
          r1 = Fp.next()
          fw.stt("dve", r1[0:64, :], kq[0:64, :], -C1, ang[0:64, :], ALU.mult, ALU.add)
          fw.stt("dve", r1[0:64, :], kq[0:64, :], -C2, r1[0:64, :], ALU.mult, ALU.add)
          fw.ts("dve", r1[0:64, :], r1[0:64, :], -PI_LO, PI_LO, ALU.max, ALU.min)
          fw.act(St, r1[0:64, :], AF.Sin)
          ra = Fp.next()
          fw.act(ra[0:64, :], r1[0:64, :], AF.Sin, scale=0.5)
          fw.tt("dve", ra[0:64, :], ra[0:64, :], ra[0:64, :], ALU.mult)
          fw.ts("dve", Ct, ra[0:64, :], -2.0, 1.0, ALU.mult, ALU.add)

          phase(1)
          for l in range(depth):
              rs = rms_to_xn(None, have_ss=(l > 0))
              for kc in range(KC):
                  fw.stt("dve", xn[kc], xT[kc], gs[:, SM_GMIX + l * 16 + kc:SM_GMIX + l * 16 + kc + 1],
                         rs, ALU.mult, ALU.mult)
              if ti == 0 and l == 0:
                  tap("xn", xn[3], [128, T], BF16)
              sl = next_slab((l, "al"))
              w = sl[:, 0:256].re("p (k c) -> p k c", c=16)
              ap_ = PS.next()
              for kc in range(KC):
                  fw.mm(ap_[0:16, :], w[:, kc, :], xn[kc], start=(kc == 0), stop=(kc == KC - 1))
              fw.copy("act", alphaT[0:16, :], ap_[0:16, :])
              def alpha_u(bi):
                  up_ = PS.next()
                  fw.mm(up_, alphaT[:, bi * 128:(bi + 1) * 128], wau[:, l * 512:(l + 1) * 512])
                  e1 = Fp.next()
                  fw.act(e1, up_, AF.Exp, scale=-1.0)
                  fw.act(spb[bi], e1, AF.Ln, bias=1.0)

              def alpha_l(bi):
                  er = PS.next()
                  fw.mm(er, Lmat, spb[bi])
                  fw.act(eke[bi], er, AF.Exp, scale=-1.0 / 16.0)
              phase(3)
              for g in range(4):
                  fw.copy("act", kTall[:, g, 0:128], kcar[l][:, g, :])
              fw.copy("act", vA[0], vcar[l])
              sl = next_slab((l, "k"))
              w = sl[:, 0:4096].re("p (k c) -> p k c", c=256)
              for g in range(4):
                  kp = PS.next()
                  for kc in range(KC):
                      fw.mm(kp[0:64, :], w[:, kc, g * 64:(g + 1) * 64], xn[kc],
                            start=(kc == 0), stop=(kc == KC - 1))
                  alpha_u(g)
                  if g > 0:
                      alpha_l(g - 1)
                  rope_store(kp[0:64, :], kTall[:, g, 128:128 + T])
              alpha_l(NB - 1)
              sl = next_slab((l, "v"))
              w = sl[:, 0:4096].re("p (k c) -> p k c", c=256)
              for bi in range(NB):
                  vp = PS.next()
                  for kc in range(KC):
                      fw.mm(vp[:, 0:256], xn[kc][:, bi * 128:(bi + 1) * 128], w[:, kc, :],
                            start=(kc == 0), stop=(kc == KC - 1))
                  fw.copy("act", vA[bi + 1], vp[:, 0:256])
              gla_w = {}

              def gla_pre(h, part):
                  eq, ek = eqk
                  if part == 0:
                      csp = PS.next()
                      for bi in range(NB):
                          fw.mm(csp[:, bi * 128:(bi + 1) * 128], spb[bi][:, h * 128:(h + 1) * 128], Umat)
                      fw.act(eq, csp, AF.Exp, scale=-1.0 / 16.0)
                      fw.act(ek, csp, AF.Exp, scale=1.0 / 16.0)
                      sl = next_slab((l, "qkb", h))
                      gla_w["qk"] = sl[:, 0:4096].re("p (k c) -> p k c", c=256)
                      w = gla_w["qk"]
                      qp = PS.next()
                      for kc in range(KC):
                          fw.mm(qp, w[:, kc, 0:128], xn[kc], start=(kc == 0), stop=(kc == KC - 1))
                      fw.stt("dve", qdec, qp, float(128 ** -0.5), eq, ALU.mult, ALU.mult)
                  elif part == 1:
                      w = gla_w["qk"]
                      kp = PS.next()
                      for kc in range(KC):
                          fw.mm(kp, w[:, kc, 128:256], xn[kc], start=(kc == 0), stop=(kc == KC - 1))
                      fw.tt("dve", kinv, kp, ek, ALU.mult)
                      fw.copy("act", kTb, kp)
                  else:
                      if part == 2:
                          sl = next_slab((l, "vb", h))
                          gla_w["v"] = sl[:, 0:4096].re("p (k c) -> p k c", c=256)
                      w = gla_w["v"]
                      for bi in ((0, 1) if part == 2 else (2, 3)):
                          vp = PS.next()
                          for kc in range(KC):
                              fw.mm(vp[:, 0:256], xn[kc][:, bi * 128:(bi + 1) * 128], w[:, kc, :],
                                    start=(kc == 0), stop=(kc == KC - 1))
                          fw.copy("act", vB[bi], vp[:, 0:256])

              def q_head(g, hh, qt, w):
                  qp = PS.next()
                  for kc in range(KC):
                      fw.mm(qp[0:64, :], w[:, kc, hh * 64:(hh + 1) * 64], xn[kc],
                            start=(kc == 0), stop=(kc == KC - 1))
                  rope_store(qp[0:64, :], qt[:, hh, :])

              def attn_scores(g, bi, qt):
                  first_blk = (ti == 0 and bi == 0)
                  js = [1] if first_blk else [0, 1]
                  sps = []
                  for j in js:
                      kcol = bi * 128 + j * 128
                      sp_ = PS.next()
                      fw.mm(sp_, kTall[:, g, kcol:kcol + 128], qt[:, :, bi * 128:(bi + 1) * 128],
                            start=True, stop=False)
                      fw.mm(sp_, ident, mprev if j == 0 else mcur, start=False, stop=True)
                      sps.append((j, sp_))
                  return sps

              def attn_finish(g, bi, sps):
                  op_ = PS.next()
                  dp_ = PS.next()
                  pjs = []
                  for j, sp_ in sps:
                      pj = Bp.next()
                      fw.act(pj, sp_, AF.Exp, scale=0.125)
                      pjs.append((j, pj))
                  for jn, (j, pj) in enumerate(pjs):
                      fw.mm(op_[0:64, :], vA[bi + j][:, g * 64:(g + 1) * 64], pj,
                            start=(jn == 0), stop=(jn == len(pjs) - 1))
                      fw.mm(dp_[0:64, :], ones[:, 0:64], pj,
                            start=(jn == 0), stop=(jn == len(pjs) - 1))
                  rd = Fp.next()
                  for hh in range(4):
                      c0 = l * 16 + g * 4 + hh
                      fw.ts("dve", rd[0:64, hh * 128:(hh + 1) * 128], dp_[0:64, hh * 128:(hh + 1) * 128],
                            es[0:64, c0:c0 + 1], None, ALU.add)
                  rr = Fp.next()
                  fw.recip(rr[0:64, :], rd[0:64, :])
                  fw.tt("dve", ya[g][:, :, bi * 128:(bi + 1) * 128],
                        op_[0:64, :].re("p (h t) -> p h t", h=4),
                        rr[0:64, :].re("p (h t) -> p h t", h=4), ALU.mult)

              phase(4)
              sl = next_slab((l, "qa", 0))
              wq = sl[:, 0:4096].re("p (k c) -> p k c", c=256)
              qt_cur = qTg.next()
              for hh in range(4):
                  q_head(0, hh, qt_cur, wq)
              if ti == 0 and l == 0:
                  tap("qT", qt_cur, [64, 4, T], BF16)
              for g in range(4):
                  if g == 1:
                      phase(5)
                  if g < 3:
                      sl = next_slab((l, "qa", g + 1))
                      wq = sl[:, 0:4096].re("p (k c) -> p k c", c=256)
                      qt_nxt = qTg.next()
                  for bi in range(NB):
                      sps = attn_scores(g, bi, qt_cur)
                      if g < 3:
                          q_head(g + 1, bi, qt_nxt, wq)
                      else:
                          gla_pre(0, bi)
                      attn_finish(g, bi, sps)
                  if g < 3:
                      qt_cur = qt_nxt
              if ti == 0 and l == 0:
                  tap("ya0", ya[0], [64, 4, T], BF16)
              for g in range(4):
                  fw.copy("act", kcar[l][:, g, :], kTall[:, g, T:T + 128])
              fw.copy("act", vcar[l], vA[NB])
              for h in range(4):
                  eq, ek = eqk
                  if h > 0:
                      for part in range(4):
                          gla_pre(h, part)
                  for bi in range(NB):
                      blk = slice(bi * 128, (bi + 1) * 128)
                      ktp = PS.next()
                      fw.mm(ktp[:, 0:128], kTb[:, blk], ident)
                      kes = []
                      for ci in range(2):
                          ke = kend.next()
                          fw.stt("dve", ke, ktp[:, 0:128], sm[:, SM_RM + ci:SM_RM + ci + 1],
                                 eke[bi][:, h * 128:(h + 1) * 128], ALU.mult, ALU.mult)
                          kes.append(ke)
                      atp = PS.next()
                      fw.mm(atp[:, 0:128], kinv[:, blk], qdec[:, blk])
                      at = attn.next()
                      fw.tt("dve", at, atp[:, 0:128], Umat, ALU.mult)
                      dsp = PS.next()
                      for ci in range(2):
                          fw.mm(dsp[:, ci * 256:(ci + 1) * 256], kes[ci], vB[bi])
                      sb_in = []
                      for ci in range(2):
                          c0 = bi * 128 + ci * 64
                          sb_in.append(Sb_cur[h])
                          fw.stt("dve", Sf[l][h], Sf[l][h], eq[:, c0 + 63:c0 + 64],
                                 dsp[:, ci * 256:(ci + 1) * 256], ALU.mult, ALU.add)
                          nb_ = Sb[h].next()
                          fw.copy("act", nb_, Sf[l][h])
                          Sb_cur[h] = nb_
                      for ci in range(2):
                          c0 = bi * 128 + ci * 64
                          for e2 in range(2):
                              fw.mm(ops[e2][:, c0:c0 + 64], vB[bi][:, e2 * 128:(e2 + 1) * 128],
                                    at[:, ci * 64:(ci + 1) * 64], start=True, stop=False)
                              fw.mm(ops[e2][:, c0:c0 + 64], sb_in[ci][:, e2 * 128:(e2 + 1) * 128],
                                    qdec[:, c0:c0 + 64], start=False, stop=True)
                  ssn = PS.next()
                  for e2 in range(2):
                      sq = Bp.next()
                      fw.act(sq, ops[e2], AF.Square)
                      fw.mm(ssn, ones, sq, start=(e2 == 0), stop=(e2 == 1))
                  tq = Fp.next()
                  fw.act(tq, ssn, AF.Sqrt, bias=float(EPS), scale=1.0 / 256.0)
                  rg = Fp.next()
                  fw.recip(rg, tq)
                  sl = next_slab((l, "rb", h))
                  w = sl[:, 0:4096].re("p (k c) -> p k c", c=256)
                  for e2 in range(2):
                      rp = PS.next()
                      for kc in range(KC):
                          fw.mm(rp, w[:, kc, e2 * 128:(e2 + 1) * 128], xn[kc],
                                start=(kc == 0), stop=(kc == KC - 1))
                      sr = Fp.next()
                      fw.act(sr, rp, AF.Silu)
                      tmp = Fp.next()
                      fw.stt("dve", tmp, ops[e2], sm[:, SM_GLAG + l * 2 + e2:SM_GLAG + l * 2 + e2 + 1],
                             rg, ALU.mult, ALU.mult)
                      fw.tt("dve", yb[h * 2 + e2], tmp, sr, ALU.mult)
              if ti == 0 and l == 0:
                  tap("yb0", yb[0], [128, T], BF16)
              for n in range(16):
                  sl = next_slab((l, "gate", n))
                  w = sl[:, 0:4096].re("p (k c) -> p k c", c=256)
                  gap = PS.next()
                  for kc in range(KC):
                      fw.mm(gap, w[:, kc, 0:128], xn[kc], start=(kc == 0), stop=(kc == KC - 1))
                  sga = Fp.next()
                  fw.act(sga, gap, AF.Sigmoid)
                  gbp = PS.next()
                  for kc in range(KC):
                      fw.mm(gbp, w[:, kc, 128:256], xn[kc], start=(kc == 0), stop=(kc == KC - 1))
                  sgb = Fp.next()
                  fw.act(sgb, gbp, AF.Sigmoid)
                  sl = next_slab((l, "wa", n))
                  w = sl[0:64, 0:2048].re("p (h c) -> p h c", c=128)
                  pap = PS.next()
                  for hh in range(16):
                      fw.mm(pap, w[:, hh, :], ya[hh // 4][:, hh % 4, :], start=(hh == 0), stop=(hh == 15))
                  t1 = Fp.next()
                  fw.tt("dve", t1, sga, pap, ALU.mult)
                  sl = next_slab((l, "wb", n))
                  w = sl[:, 0:1024].re("p (h c) -> p h c", c=128)
                  pbp = PS.next()
                  for ec in range(8):
                      fw.mm(pbp, w[:, ec, :], yb[ec], start=(ec == 0), stop=(ec == 7))
                  t2 = Fp.next()
                  fw.tt("dve", t2, sgb, pbp, ALU.mult)
                  fw.tt("dve", mixed[n], t1, t2, ALU.add)
              if ti == 0 and l == 0:
                  tap("mixed0", mixed[0], [128, T], BF16)
              phase(9)
              for n2 in range(16):
                  sl = next_slab((l, "wo", n2))
                  w = sl[:, 0:2048].re("p (k c) -> p k c", c=128)
                  yp = PS.next()
                  for n in range(16):
                      fw.mm(yp, w[:, n, :], mixed[n], start=(n == 0), stop=(n == 15))
                  fw.tt("dve", xT[n2], xT[n2], yp, ALU.add)
                  if n2 >= 2:
                      ss_add(n2 - 2)
              ss_add(14)
              ss_add(15)
              if ti == 0 and l == 0:
                  tap("xmid", xT[0], [128, T], F32)
              phase(10)
              rs = rms_to_xn(None, have_ss=True)
              for kc in range(KC):
                  fw.stt("dve", xn[kc], xT[kc], gs[:, SM_GFFN + l * 16 + kc:SM_GFFN + l * 16 + kc + 1],
                         rs, ALU.mult, ALU.mult)
              for gi in range(NG):
                  for j in range(GFF):
                      i = gi * GFF + j
                      sl = next_slab((l, "up", i))
                      w = sl[:, 0:4096].re("p (k c) -> p k c", c=256)
                      cv = []
                      for part in range(2):
                          ch = part * NFF + i
                          hp = PS.next()
                          for kc in range(KC):
                              fw.mm(hp, w[:, kc, part * 128:(part + 1) * 128], xn[kc],
                                    start=(kc == 0), stop=(kc == KC - 1))
                          hb = Hp.next()
                          fw.copy("dve", hb[:, 0:2], carry[l][:, ch, :])
                          fw.copy("act", hb[:, 2:T + 2], hp)
                          fw.copy("dve", carry[l][:, ch, :], hb[:, T:T + 2])
                          cb = SM_CONV + l * 352 + ch * 4
                          acc = Fp.next()
                          fw.ts("dve", acc, hb[:, 2:T + 2], sm[:, cb + 2:cb + 3], sm[:, cb + 3:cb + 4],
                                ALU.mult, ALU.add)
                          fw.stt("dve", acc, hb[:, 1:T + 1], sm[:, cb + 1:cb + 2], acc, ALU.mult, ALU.add)
                          fw.stt("dve", acc, hb[:, 0:T], sm[:, cb:cb + 1], acc, ALU.mult, ALU.add)
                          cv.append(acc)
                      sg = Fp.next()
                      fw.act(sg, cv[0], AF.Silu)
                      fw.tt("dve", aT[j], sg, cv[1], ALU.mult)
                      if ti == 0 and l == 0 and i == 0:
                          tap("a0", aT[0], [128, T], BF16)
                  for n2 in range(16):
                      sl = next_slab((l, "dn", gi, n2))
                      w = sl[:, 0:GFF * 128].re("p (j c) -> p j c", c=128)
                      yp = PS.next()
                      for j in range(GFF):
                          fw.mm(yp, w[:, j, :], aT[j], start=(j == 0), stop=(j == GFF - 1))
                      fw.tt("dve", xT[n2], xT[n2], yp, ALU.add)
                      if gi == NG - 1 and n2 >= 2:
                          ss_add(n2 - 2)
                  if gi == NG - 1:
                      ss_add(14)
                      ss_add(15)
              if ti == 0 and l == 0:
                  tap("xout", xT[0], [128, T], F32)
              phase(11)
              if depth > 1:
                  nl = (l + 1) % depth
                  for h in range(4):
                      nb_ = Sb[h].next()
                      fw.copy("act", nb_, Sf[nl][h])
                      Sb_cur[h] = nb_
          rs = rms_to_xn(None, have_ss=True)
          for kc in range(KC):
              o = Fp.next()
              if o is rs:
                  o = Fp.next()
              fw.stt("dve", o, xT[kc], gs[:, SM_GFIN + kc:SM_GFIN + kc + 1], rs, ALU.mult, ALU.mult)
              fw.dma("sp", yT_d[kc, :, t0:t0 + T], o, osem[(Fp.i - 1) % len(Fp.bufs)])
    except _Stop:
        pass
    assert kstop or state["used"] == len(seq)
    fw.wait_all("sp", Fp.bufs + tapbufs)
    print("n_inst", fw.n_inst, "n_wait", fw.n_wait)
    fw.finalize()
    return nc, tap_d


_CACHE = {}


def prepare_inputs(x, positions, norm_mix_g, w_in, sink, w_alpha_up, b_alpha, gla_norm_g,
                   w_branch_a, w_branch_b, w_out, norm_ffn_g, w_up, conv_w, conv_b, w_down,
                   final_g, depth=DEPTH, cores=None):
    cores = list(range(NCORE)) if cores is None else cores
    wpack, _ = pack_weights(w_in, w_branch_a, w_branch_b, w_out, w_up, w_down, depth)
    sm = make_sm(norm_mix_g, norm_ffn_g, final_g, gla_norm_g, conv_w, conv_b, sink, depth)
    cst = make_consts()
    wau = np.zeros((32, depth * 512), np.float32)
    for l in range(depth):
        wau[0:16, l * 512:(l + 1) * 512] = w_alpha_up[l]
        wau[16, l * 512:(l + 1) * 512] = b_alpha[l]
    in_maps = []
    for b in cores:
        xT = np.ascontiguousarray(np.asarray(x[b], np.float32).T.reshape(KC, 128, S))
        posr = np.ascontiguousarray(np.broadcast_to(np.asarray(positions[b], np.int32)[None, :], (128, S)))
        in_maps.append({"xT": xT, "posr": posr, "wpack": wpack, "sm": sm, "cst": cst, "wau": wau})
    return in_maps


def kernel(**inputs):
    inputs = {k: np.asarray(v) for k, v in inputs.items()}
    in_maps = prepare_inputs(**inputs)
    if "nc" not in _CACHE:
        _CACHE["nc"] = build()[0]
    nc = _CACHE["nc"]
    res = run_bass_kernel_spmd(nc, in_maps, core_ids=list(range(NCORE)))
    out = np.empty((NCORE, S, D), np.float32)
    for b in range(NCORE):
        yT = res.results[b]["yT"]
        out[b] = yT.reshape(D, S).T
    return out
```
